# Optimizing a Trainium2 kernel written in Bass

```python
import jax, jax.numpy as jnp
from jax import lax
import numpy as np

D_MODEL = 2048
BATCH = 1
SEQ = 8192
DEPTH = 2

CHUNK = 64
D_MIX = D_MODEL
A_WIDTH = D_MIX // 4
A_HEAD_DIM = 128
A_HEADS = A_WIDTH // A_HEAD_DIM
A_BLOCK = 128
B_WIDTH = D_MIX // 4
POOL_WINDOWS = (2, 4, 8, 16)
POOL_GROUPS = len(POOL_WINDOWS)
B_GROUP = B_WIDTH // POOL_GROUPS
C_WIDTH = D_MIX - A_WIDTH - B_WIDTH
C_HEAD_DIM = 128
C_HEADS = C_WIDTH // C_HEAD_DIM
Q_BLOCK = 128
N_IN = 2 * A_WIDTH + B_WIDTH + 3 * C_WIDTH + C_HEADS
SPLITS = (A_WIDTH, 2 * A_WIDTH, 2 * A_WIDTH + B_WIDTH,
          2 * A_WIDTH + B_WIDTH + C_WIDTH, 2 * A_WIDTH + B_WIDTH + 2 * C_WIDTH,
          2 * A_WIDTH + B_WIDTH + 3 * C_WIDTH)
N_EXPERTS = 16
N_GROUPS = 4
EXPERTS_PER_GROUP = N_EXPERTS // N_GROUPS
TOPK_GROUP = 1
TOP_K = 2
D_EXPERT = D_MODEL // 2
MOE_BLOCK = 128
EPS = 1e-6

kernel_name = 'hybrid_gmlp_pool_fox_moe_encoder'


def rms_norm(x, gain):
    xf = x.astype(jnp.float32)
    y = xf * lax.rsqrt(jnp.mean(xf * xf, axis=-1, keepdims=True) + EPS)
    return (y * gain.astype(jnp.float32)).astype(x.dtype)


def gmlp_mixer(u, v, ws, bs, vg):
    B, S, _ = u.shape
    u = jax.nn.gelu(u)
    v = rms_norm(jax.nn.gelu(v).reshape(B, S, A_HEADS, A_HEAD_DIM), vg)
    vb = v.reshape(B, S // A_BLOCK, A_BLOCK, A_HEADS, A_HEAD_DIM)
    cid = jnp.arange(A_BLOCK) // CHUNK
    mask = cid[None, :] <= cid[:, None]
    wm = jnp.where(mask[None], ws, 0).astype(v.dtype)
    sp = jnp.einsum('hts,bnshd->bnthd', wm, vb) + bs.T.astype(v.dtype)[None, None, :, :, None]
    return u * sp.reshape(B, S, A_WIDTH)


def pool_mixer(p, bw, bscale):
    B, S, _ = p.shape
    pf = p.astype(jnp.float32)
    cs = jnp.concatenate([jnp.zeros((B, 1, B_WIDTH), jnp.float32), jnp.cumsum(pf, axis=1)], axis=1)
    t1 = jnp.arange(1, S + 1)
    outs = []
    for g, w in enumerate(POOL_WINDOWS):
        sl = slice(g * B_GROUP, (g + 1) * B_GROUP)
        csg = cs[..., sl]
        lag = jnp.pad(csg, ((0, 0), (w, 0), (0, 0)))[:, :S + 1]
        win_sum = csg[:, 1:] - lag[:, 1:]
        cnt = jnp.minimum(t1, w).astype(jnp.float32)[None, :, None]
        outs.append(win_sum / cnt - pf[..., sl])
    d = jnp.stack(outs, axis=2).astype(p.dtype)
    y = jnp.einsum('bsgc,gce->bsge', d, bw)
    return y.reshape(B, S, B_WIDTH) * bscale


def forgetting_attention(q, k, v, fz, qg, kg, bf):
    B, S, _ = q.shape
    q = rms_norm(q.reshape(B, S, C_HEADS, C_HEAD_DIM), qg)
    k = rms_norm(k.reshape(B, S, C_HEADS, C_HEAD_DIM), kg)
    v = v.reshape(B, S, C_HEADS, C_HEAD_DIM)
    log_f = jax.nn.log_sigmoid(fz.astype(jnp.float32) + bf.astype(jnp.float32))
    F = jnp.cumsum(log_f, axis=1)
    Fk = jnp.transpose(F, (0, 2, 1))[:, :, None, :]
    nq = S // Q_BLOCK
    qb = q.reshape(B, nq, Q_BLOCK, C_HEADS, C_HEAD_DIM).transpose(1, 0, 2, 3, 4)
    Fq = F.reshape(B, nq, Q_BLOCK, C_HEADS).transpose(1, 0, 2, 3)
    kpos = jnp.arange(S)
    scale = C_HEAD_DIM ** -0.5

    def block(args):
        qi, Fi, i = args
        qpos = i * Q_BLOCK + jnp.arange(Q_BLOCK)
        s = jnp.einsum('bqhd,bkhd->bhqk', qi, k, preferred_element_type=jnp.float32) * scale
        s = s + jnp.transpose(Fi, (0, 2, 1))[..., None] - Fk
        s = jnp.where((kpos[None, :] <= qpos[:, None])[None, None], s, -jnp.inf)
        pr = jax.nn.softmax(s, axis=-1).astype(v.dtype)
        return jnp.einsum('bhqk,bkhd->bqhd', pr, v)

    out = lax.map(block, (qb, Fq, jnp.arange(nq)))
    return out.transpose(1, 0, 2, 3, 4).reshape(B, S, C_WIDTH)


def token_mixer(h, w_in, a_ws, a_bs, a_vg, b_w, b_scale, c_qg, c_kg, c_bf, w_out):
    z = h @ w_in
    u, v, p, q, k, va, fz = jnp.split(z, SPLITS, axis=-1)
    y_a = gmlp_mixer(u, v, a_ws, a_bs, a_vg)
    y_b = pool_mixer(p, b_w, b_scale)
    y_c = forgetting_attention(q, k, va, fz, c_qg, c_kg, c_bf)
    return jnp.concatenate([y_a, y_b, y_c], axis=-1) @ w_out


def route(ht, w_router, b_router):
    T = ht.shape[0]
    logits = jnp.einsum('td,de->te', ht, w_router, preferred_element_type=jnp.float32)
    scores = jax.nn.sigmoid(logits)
    sel = scores + b_router.astype(jnp.float32)
    grp = sel.reshape(T, N_GROUPS, EXPERTS_PER_GROUP)
    grp_score = lax.top_k(grp, TOP_K)[0].sum(-1)
    _, gidx = lax.top_k(grp_score, TOPK_GROUP)
    gmask = jnp.any(gidx[..., None] == jnp.arange(N_GROUPS), axis=1)
    sel = jnp.where(jnp.repeat(gmask, EXPERTS_PER_GROUP, axis=1), sel, -jnp.inf)
    _, eidx = lax.top_k(sel, TOP_K)
    gw = jnp.take_along_axis(scores, eidx, axis=1)
    gw = gw / jnp.sum(gw, axis=-1, keepdims=True)
    return eidx, gw


def moe_ffn(h, w_router, b_router, w_gate, w_up, w_down):
    B, S, D = h.shape
    T = B * S
    ht = h.reshape(T, D)
    eidx, gw = route(ht, w_router, b_router)
    A = T * TOP_K
    flat_e = eidx.reshape(A)
    flat_tok = jnp.repeat(jnp.arange(T, dtype=jnp.int32), TOP_K)
    flat_w = gw.reshape(A)
    order = jnp.argsort(flat_e)
    sorted_e = flat_e[order]
    counts = jnp.bincount(flat_e, length=N_EXPERTS)
    padded = (counts + MOE_BLOCK - 1) // MOE_BLOCK * MOE_BLOCK
    ends = jnp.cumsum(padded)
    pad_start = ends - padded
    start = jnp.cumsum(counts) - counts
    dest = pad_start[sorted_e] + jnp.arange(A) - start[sorted_e]
    P = A + N_EXPERTS * MOE_BLOCK
    n_blocks = P // MOE_BLOCK
    row_tok = jnp.zeros((P,), jnp.int32).at[dest].set(flat_tok[order])
    row_gate = jnp.zeros((P,), jnp.float32).at[dest].set(flat_w[order])
    block_start = jnp.arange(n_blocks) * MOE_BLOCK
    block_expert = jnp.minimum(jnp.searchsorted(ends, block_start, side='right'), N_EXPERTS - 1).astype(jnp.int32)
    xs = ht[row_tok].reshape(n_blocks, MOE_BLOCK, D)

    def run(args):
        xb, e = args
        hid = jax.nn.silu(xb @ w_gate[e]) * (xb @ w_up[e])
        return hid @ w_down[e]

    yb = lax.map(run, (xs, block_expert)).reshape(P, D)
    out = jnp.zeros((T, D), h.dtype).at[row_tok].add((yb * row_gate[:, None]).astype(h.dtype))
    return out.reshape(B, S, D)


def setup_inputs(seed: int = 0) -> dict:
    key = jax.random.key(seed)
    ks = jax.random.split(key, 24)
    f32 = jnp.float32
    L, D = DEPTH, D_MODEL

    def nrm(k, shape, scale):
        return jax.random.normal(k, shape, f32) * scale

    return {
        'x': nrm(ks[0], (BATCH, SEQ, D), 1.0),
        'c': nrm(ks[1], (BATCH, D), 1.0),
        'w_ada': nrm(ks[2], (L, D, 6 * D), 0.5 * D ** -0.5),
        'b_ada': nrm(ks[3], (L, 6 * D), 0.02),
        'g_mix': 1.0 + nrm(ks[4], (L, D), 0.02),
        'g_ffn': 1.0 + nrm(ks[5], (L, D), 0.02),
        'w_in': nrm(ks[6], (L, D, N_IN), D ** -0.5),
        'a_ws': nrm(ks[7], (L, A_HEADS, A_BLOCK, A_BLOCK), 0.5 * A_BLOCK ** -0.5),
        'a_bs': 1.0 + nrm(ks[8], (L, A_HEADS, A_BLOCK), 0.02),
        'a_vg': 1.0 + nrm(ks[9], (L, A_HEADS, A_HEAD_DIM), 0.02),
        'b_w': nrm(ks[10], (L, POOL_GROUPS, B_GROUP, B_GROUP), B_GROUP ** -0.5),
        'b_scale': 1.0 + nrm(ks[11], (L, B_WIDTH), 0.02),
        'c_qg': 1.0 + nrm(ks[12], (L, C_HEAD_DIM), 0.02),
        'c_kg': 1.0 + nrm(ks[13], (L, C_HEAD_DIM), 0.02),
        'c_bf': jnp.linspace(0.0, 5.0, C_HEADS, dtype=f32)[None, :] + nrm(ks[14], (L, C_HEADS), 0.1),
        'w_out': nrm(ks[15], (L, D_MIX, D), D_MIX ** -0.5),
        'w_router': nrm(ks[16], (D, N_EXPERTS), D ** -0.5),
        'b_router': nrm(ks[17], (N_EXPERTS,), 0.01),
        'e_gate': nrm(ks[18], (L, N_EXPERTS, D, D_EXPERT), D ** -0.5),
        'e_up': nrm(ks[19], (L, N_EXPERTS, D, D_EXPERT), D ** -0.5),
        'e_down': nrm(ks[20], (L, N_EXPERTS, D_EXPERT, D), D_EXPERT ** -0.5),
    }


def reference(x, c, w_ada, b_ada, g_mix, g_ffn, w_in, a_ws, a_bs, a_vg, b_w, b_scale,
              c_qg, c_kg, c_bf, w_out, w_router, b_router, e_gate, e_up, e_down):
    c_act = jax.nn.silu(c)
    for l in range(DEPTH):
        mod = c_act @ w_ada[l] + b_ada[l]
        sh1, sc1, g1, sh2, sc2, g2 = [m[:, None, :] for m in jnp.split(mod, 6, axis=-1)]
        h = rms_norm(x, g_mix[l]) * (1 + sc1) + sh1
        y = token_mixer(h, w_in[l], a_ws[l], a_bs[l], a_vg[l], b_w[l], b_scale[l],
                        c_qg[l], c_kg[l], c_bf[l], w_out[l])
        x = x + g1 * y
        h = rms_norm(x, g_ffn[l]) * (1 + sc2) + sh2
        x = x + g2 * moe_ffn(h, w_router, b_router, e_gate[l], e_up[l], e_down[l])
    return x
```

```python
import numpy as np
from contextlib import ExitStack
import ml_dtypes
import concourse.bass as bass
import concourse.mybir as mybir
from concourse.bass_utils import run_bass_kernel_spmd

F32 = mybir.dt.float32
BF16 = mybir.dt.bfloat16
AF = mybir.ActivationFunctionType
ALU = mybir.AluOpType
AX = mybir.AxisListType

NCORES = 8
D = 2048
SEQ = 8192
NT = SEQ // NCORES
NTL = NT // 128
HL = 16
NIN = 4616
NE = 16
DE = 1024
EPS = 1e-6
NEG = -30000.0

SAME_ENGINE_SYNC = True
DBG = {'phase1': True, 'ngroups': 99, 'pool': True, 'logsig': True, 'ntiles': 99, 'qk': 9}


class Res:
    __slots__ = ("name", "writer", "readers", "dsem", "dcount", "psum")

    def __init__(self, name):
        self.name = name
        self.writer = None
        self.readers = []
        self.dsem = None
        self.dcount = 0
        self.psum = False


class Eng:
    def __init__(self, name, sem):
        self.name = name
        self.sem = sem
        self.count = 0
        self.ops = []
        self.seen = {}


class Prog:
    def __init__(self, nc, stack):
        self.nc = nc
        self.stack = stack
        self.engs = {}
        for n in ("tensor", "vector", "scalar", "gpsimd", "sync"):
            sem = stack.enter_context(nc.semaphore("prog_" + n))
            self.engs[n] = Eng(n, sem)
        self.nres = 0
        self.all_res = []

    def res(self, name=None):
        self.nres += 1
        r = Res((name or "r") + f"_{self.nres}")
        self.all_res.append(r)
        return r

    def sb(self, name, shape, dtype):
        t = self.stack.enter_context(self.nc.sbuf_tensor(name, list(shape), dtype))
        return t, self.res(name)

    def ps(self, name, shape, dtype):
        t = self.stack.enter_context(self.nc.psum_tensor(name, list(shape), dtype))
        r = self.res(name)
        r.psum = True
        return t, r

    def _deps(self, E, reads, writes):
        toks = []
        for r in reads:
            if r.writer is not None:
                toks.append(r.writer)
        for w in writes:
            if w.writer is not None:
                toks.append(w.writer)
            toks.extend(w.readers)
        best = {}
        for (s, v) in toks:
            if best.get(id(s), (None, 0))[1] < v:
                best[id(s)] = (s, v)
        waits = []
        for (s, v) in best.values():
            if s is E.sem:
                if (not SAME_ENGINE_SYNC) or E.name == "tensor":
                    continue
            if E.seen.get(id(s), 0) >= v:
                continue
            E.seen[id(s)] = v
            waits.append((s, v))
        return waits

    def _record(self, tok, reads, writes):
        for r in reads:
            r.readers.append(tok)
        for w in writes:
            w.writer = tok
            w.readers = []

    def op(self, eng, fn, reads=(), writes=()):
        writes = list(writes) + [r for r in reads if r.psum]
        reads = [r for r in reads if not r.psum]
        E = self.engs[eng]
        waits = self._deps(E, reads, writes)
        E.count += 1
        tok = (E.sem, E.count)
        E.ops.append((waits, fn, (E.sem, 1)))
        self._record(tok, reads, writes)
        return tok

    def dma(self, eng, fn, owner, reads=(), writes=()):
        E = self.engs[eng]
        waits = self._deps(E, reads, writes)
        if owner.dsem is None:
            owner.dsem = self.stack.enter_context(self.nc.semaphore("d_" + owner.name))
        owner.dcount += 16
        tok = (owner.dsem, owner.dcount)
        E.ops.append((waits, fn, (owner.dsem, 16)))
        self._record(tok, reads, writes)
        return tok

    def finish(self, eng="sync"):
        E = self.engs[eng]
        waits = self._deps(E, [], self.all_res)
        E.ops.append((waits, None, None))

    def emit(self):
        nc = self.nc
        with nc.Block() as block:
            def run(E):
                def body(e):
                    for waits, fn, inc in E.ops:
                        for (s, v) in waits:
                            e.wait_ge(s, v)
                        if fn is not None:
                            fn(e).then_inc(inc[0], inc[1])
                return body
            block.tensor(run(self.engs["tensor"]))
            block.vector(run(self.engs["vector"]))
            block.scalar(run(self.engs["scalar"]))
            block.gpsimd(run(self.engs["gpsimd"]))
            block.sync(run(self.engs["sync"]))


def _dram(nc, name, shape, dtype, kind):
    return nc.dram_tensor(name, list(shape), dtype, kind=kind).ap()


MODC = 12288 // NCORES


def build_mod():
    nc = bass.Bass("TRN2", target_bir_lowering=False)
    ccol = _dram(nc, "ccol", [128, 16], F32, "ExternalInput")
    wada = _dram(nc, "wada", [2, D, MODC], F32, "ExternalInput")
    bada = _dram(nc, "bada", [2, MODC], F32, "ExternalInput")
    modo = _dram(nc, "modo", [2, MODC], F32, "ExternalOutput")
    with ExitStack() as st:
        P = Prog(nc, st)
        cc, r_cc = P.sb("cc", [128, 16], F32)
        ca, r_ca = P.sb("ca", [128, 16], F32)
        P.dma("sync", lambda e: e.dma_start(out=cc[:], in_=ccol), r_cc, writes=[r_cc])
        P.op("scalar", lambda e: e.activation(ca[:], cc[:], AF.Silu), reads=[r_cc], writes=[r_ca])
        ps, r_ps = P.ps("ps", [1, MODC], F32)
        bufs = [P.sb(f"wb{i}", [128, 8, MODC], F32) for i in range(2)]
        bb, r_bb = P.sb("bb", [1, 2 * MODC], F32)
        mo, r_mo = P.sb("mo", [1, 2 * MODC], F32)
        P.dma("sync", lambda e: e.dma_start(out=bb[:], in_=bada.rearrange("l n -> (l n)").rearrange("(o n) -> o n", o=1)), r_bb, writes=[r_bb])
        for l in range(2):
            for hf in range(2):
                wt, r_wt = bufs[hf]
                src = wada[l, hf * 1024:(hf + 1) * 1024, :].rearrange("(j p) n -> p j n", p=128)
                for q in range(2):
                    P.dma("sync" if q == 0 else "gpsimd",
                          lambda e, wt=wt, src=src, q=q: e.dma_start(out=wt[:, q * 4:(q + 1) * 4, :], in_=src[:, q * 4:(q + 1) * 4, :]),
                          r_wt, writes=[r_wt])
            for nb in range(3):
                for j in range(16):
                    wt, r_wt = bufs[j // 8]
                    P.op("tensor", lambda e, wt=wt, j=j, nb=nb: e.matmul(
                        ps[0:1, nb * 512:(nb + 1) * 512], ca[:, j:j + 1], wt[:, j % 8, nb * 512:(nb + 1) * 512],
                        start=(j == 0), stop=(j == 15)), reads=[r_ca, r_wt], writes=[r_ps])
            P.op("vector", lambda e, l=l: e.tensor_tensor(mo[0:1, l * MODC:(l + 1) * MODC], ps[0:1, :], bb[0:1, l * MODC:(l + 1) * MODC], ALU.add),
                 reads=[r_ps, r_bb], writes=[r_mo])
        P.dma("sync", lambda e: e.dma_start(out=modo.rearrange("l n -> (l n)").rearrange("(o n) -> o n", o=1), in_=mo[:]), r_mo, reads=[r_mo])
        P.finish()
        P.emit()
    return nc


def build_A():
    nc = bass.Bass("TRN2", target_bir_lowering=False)
    I = "ExternalInput"
    O = "ExternalOutput"
    x = _dram(nc, "x", [NT, D], F32, I)
    xh = _dram(nc, "xh", [HL, D], F32, I)
    modc = _dram(nc, "modc", [128, 3, 16], F32, I)
    win = _dram(nc, "win", [D, NIN], F32, I)
    identd = _dram(nc, "ident", [128, 128], F32, I)
    awsT = _dram(nc, "awsT", [4, 128, 128], F32, I)
    absd = _dram(nc, "abs", [1, 512], F32, I)
    avgd = _dram(nc, "avg", [1, 512], F32, I)
    bwd = _dram(nc, "bw", [4, 128, 128], F32, I)
    bscd = _dram(nc, "bsc", [128, 4], F32, I)
    qkg = _dram(nc, "qkg", [128, 2], F32, I)
    cbf = _dram(nc, "cbf", [1, 8], F32, I)
    hmaskd = _dram(nc, "hmask", [128, 4, HL], F32, I)
    invcd = _dram(nc, "invc", [128, 4, HL], F32, I)
    qTo = _dram(nc, "qT", [8, 128, NT], BF16, O)
    kTo = _dram(nc, "kT", [8, 128, NT], BF16, O)
    vo = _dram(nc, "v", [NT, 1024], BF16, O)
    lfo = _dram(nc, "logf", [NT, 8], F32, O)
    yabo = _dram(nc, "yab", [8, 128, NT], BF16, O)

    with ExitStack() as st:
        P = Prog(nc, st)
        TW = HL + NT
        ident, r_ident = P.sb("identb", [128, 128], BF16)
        P.dma("gpsimd", lambda e: e.dma_start(out=ident[:], in_=identd), r_ident, writes=[r_ident])
        ones, r_ones = P.sb("ones", [128, 128], BF16)
        P.op("vector", lambda e: e.memset(ones[:], 1.0), writes=[r_ones])
        mc, r_mc = P.sb("mc", [128, 3, 16], F32)
        P.dma("sync", lambda e: e.dma_start(out=mc[:], in_=modc), r_mc, writes=[r_mc])
        a1, r_a1 = P.sb("a1", [128, 16], F32)
        P.op("vector", lambda e: e.scalar_tensor_tensor(a1[:], mc[:, 1, :], 1.0, mc[:, 2, :], ALU.add, ALU.mult),
             reads=[r_mc], writes=[r_a1])
        wsT, r_wsT = P.sb("wsT", [128, 4, 128], BF16)
        P.dma("gpsimd", lambda e: e.dma_start(out=wsT[:], in_=awsT.rearrange("h s t -> s h t")), r_wsT, writes=[r_wsT])
        P.op("vector", lambda e: e.memset(wsT[64:128, :, 0:64], 0.0), writes=[r_wsT])
        bsB, r_bsB = P.sb("bsB", [128, 512], F32)
        P.dma("sync", lambda e: e.dma_start(out=bsB[:], in_=absd.partition_broadcast(128)), r_bsB, writes=[r_bsB])
        vgB, r_vgB = P.sb("vgB", [128, 512], F32)
        P.dma("sync", lambda e: e.dma_start(out=vgB[:], in_=avgd.partition_broadcast(128)), r_vgB, writes=[r_vgB])
        bw, r_bw = P.sb("bwb", [128, 4, 128], BF16)
        P.dma("gpsimd", lambda e: e.dma_start(out=bw[:], in_=bwd.rearrange("g c e -> c g e")), r_bw, writes=[r_bw])
        bsc, r_bsc = P.sb("bscs", [128, 4], F32)
        P.dma("sync", lambda e: e.dma_start(out=bsc[:], in_=bscd), r_bsc, writes=[r_bsc])
        qk, r_qk = P.sb("qks", [128, 2], F32)
        P.dma("sync", lambda e: e.dma_start(out=qk[:], in_=qkg), r_qk, writes=[r_qk])
        P.op("vector", lambda e: e.tensor_scalar(qk[:, 0:1], qk[:, 0:1], 128.0 ** -0.5, None, ALU.mult), reads=[r_qk], writes=[r_qk])
        bfB, r_bfB = P.sb("bfB", [128, 8], F32)
        P.dma("sync", lambda e: e.dma_start(out=bfB[:], in_=cbf.partition_broadcast(128)), r_bfB, writes=[r_bfB])
        hmask, r_hmask = P.sb("hmasks", [128, 4, HL], F32)
        P.dma("sync", lambda e: e.dma_start(out=hmask[:], in_=hmaskd), r_hmask, writes=[r_hmask])
        invc, r_invc = P.sb("invcs", [128, 4, HL], F32)
        P.dma("sync", lambda e: e.dma_start(out=invc[:], in_=invcd), r_invc, writes=[r_invc])

        groups = [("p", 1024), ("q", 1536), ("q", 2048), ("k", 2560), ("k", 3072), ("u", 0), ("v", 512),
                  ("va", 3584), ("va", 4096)]
        wg = [P.sb(f"wg{i}", [128, 16, 512], BF16) for i in range(2)]
        wfz, r_wfz = P.sb("wfz", [128, 16, 8], BF16)
        winr = win.rearrange("(j p) n -> p j n", p=128)

        def load_group(gi):
            wt, r_wt = wg[gi % 2]
            c0 = groups[gi][1]
            for q in range(4):
                P.dma("gpsimd", lambda e, wt=wt, c0=c0, q=q: e.dma_start(
                    out=wt[:, q * 4:(q + 1) * 4, :], in_=winr[:, q * 4:(q + 1) * 4, c0:c0 + 512]), r_wt, writes=[r_wt])

        load_group(0)
        load_group(1)
        P.dma("gpsimd", lambda e: e.dma_start(out=wfz[:], in_=winr[:, :, 4608:4616]), r_wfz, writes=[r_wfz])

        hT, r_hT = P.sb("hT", [128, 16, TW], BF16)
        r_hTt = [P.res(f"hTt{i}") for i in range(NTL + 1)]
        xt = [P.sb(f"xt{i}", [128, D], F32) for i in range(2)]
        xn = [P.sb(f"xn{i}", [128, D], BF16) for i in range(2)]
        ss = [P.sb(f"ss{i}", [128, 2], F32) for i in range(2)]
        tp = [P.ps(f"tp{i}", [128, 1024], BF16) for i in range(2)]
        tpi = 0
        for ti in range(NTL + 1):
            if not DBG['phase1'] or ti >= DBG['ntiles']:
                break
            npart = HL if ti == 0 else 128
            col0 = 0 if ti == 0 else HL + (ti - 1) * 128
            src = xh if ti == 0 else x[(ti - 1) * 128:ti * 128, :]
            xtt, r_xt = xt[ti % 2]
            xnt, r_xn = xn[ti % 2]
            sst, r_ss = ss[ti % 2]
            P.dma("sync", lambda e, xtt=xtt, src=src, npart=npart: e.dma_start(out=xtt[0:npart, :], in_=src), r_xt, writes=[r_xt])
            P.op("vector", lambda e, sst=sst: e.memset(sst[:], 0.0), writes=[r_ss])
            P.op("scalar", lambda e, xtt=xtt, sst=sst, npart=npart: e.activation(
                xnt[0:npart, :], xtt[0:npart, :], AF.Square, accum_out=sst[0:npart, 0:1]),
                reads=[r_xt, r_ss], writes=[r_xn, r_ss])
            P.op("scalar", lambda e, sst=sst, npart=npart: e.activation(
                sst[0:npart, 1:2], sst[0:npart, 0:1], AF.Sqrt, bias=EPS, scale=1.0 / D), reads=[r_ss], writes=[r_ss])
            P.op("vector", lambda e, sst=sst, npart=npart: e.reciprocal(sst[0:npart, 1:2], sst[0:npart, 1:2]), reads=[r_ss], writes=[r_ss])
            P.op("vector", lambda e, xtt=xtt, xnt=xnt, sst=sst, npart=npart: e.tensor_scalar(
                xnt[0:npart, :], xtt[0:npart, :], sst[0:npart, 1:2], None, ALU.mult), reads=[r_xt, r_ss], writes=[r_xn])
            for half in range(2):
                tpt, r_tp = tp[tpi % 2]
                tpi += 1
                for jj in range(8):
                    j = half * 8 + jj
                    P.op("tensor", lambda e, tpt=tpt, xnt=xnt, j=j, jj=jj, npart=npart: e.transpose(
                        tpt[:, jj * 128:jj * 128 + npart], xnt[0:npart, j * 128:(j + 1) * 128], ident[0:npart, 0:npart]),
                        reads=[r_xn, r_ident], writes=[r_tp])
                for jj in range(8):
                    j = half * 8 + jj
                    if half == 0:
                        P.op("vector", lambda e, tpt=tpt, j=j, jj=jj, npart=npart, col0=col0: e.tensor_scalar(
                            hT[:, j, col0:col0 + npart], tpt[:, jj * 128:jj * 128 + npart], a1[:, j:j + 1], mc[:, 0, j:j + 1],
                            ALU.mult, ALU.add), reads=[r_tp, r_a1, r_mc], writes=[r_hTt[ti]])
                    else:
                        P.op("scalar", lambda e, tpt=tpt, j=j, jj=jj, npart=npart, col0=col0: e.activation(
                            hT[:, j, col0:col0 + npart], tpt[:, jj * 128:jj * 128 + npart], AF.Identity,
                            bias=mc[:, 0, j:j + 1], scale=a1[:, j:j + 1]), reads=[r_tp, r_a1, r_mc], writes=[r_hTt[ti]])

        acc = [P.ps(f"acc{i}", [128, 512], F32) for i in range(3)]
        aux = [P.ps(f"aux{i}", [128, 512], F32) for i in range(2)]
        acci = [0]
        auxi = [0]

        def next_acc():
            a = acc[acci[0] % 3]
            acci[0] += 1
            return a

        def next_aux():
            a = aux[auxi[0] % 2]
            auxi[0] += 1
            return a

        pT, r_pT = P.sb("pT", [128, 4, TW], F32)
        qkT, r_qkT = P.sb("qkT", [128, 16, NT], BF16)
        guT, r_guT = P.sb("guT", [128, 4, NT], BF16)
        yab, r_yab = P.sb("yabs", [128, 8, NT], BF16)
        vsb, r_vsb = P.sb("vsb", [128, NTL, 1024], BF16)
        fz, r_fz = P.sb("fzs", [128, NTL, 8], F32)
        sq = [P.sb(f"sq{i}", [128, 512], BF16) for i in range(2)]
        raw = [P.sb(f"raw{i}", [128, 512], F32) for i in range(2)]
        rst = [P.sb(f"rst{i}", [128, 512], F32) for i in range(2)]
        gv, r_gv = P.sb("gv", [128, 512], F32)
        gv2, r_gv2 = P.sb("gv2", [128, 512], F32)
        vn, r_vn = P.sb("vn", [128, 512], BF16)
        vs4, r_vs4 = P.sb("vs4", [128, 8], F32)
        spt, r_spt = P.sb("spt", [128, 512], F32)
        cnt = [0]

        def feature_major(gi, wt, r_wt):
            kind, c0 = groups[gi]
            for c in range(4):
                col = c0 + c * 128
                for th in range(2):
                    a, r_a = next_acc()
                    t0 = HL + th * 512
                    for j in range(16):
                        P.op("tensor", lambda e, a=a, wt=wt, c=c, j=j, t0=t0: e.matmul(
                            a[:, :], wt[:, j, c * 128:(c + 1) * 128], hT[:, j, t0:t0 + 512], start=(j == 0), stop=(j == 15)),
                            reads=[r_wt] + r_hTt[1 + th * 4:5 + th * 4], writes=[r_a])
                    if kind == "p":
                        P.op("scalar", lambda e, a=a, c=c, t0=t0: e.copy(pT[:, c, t0:t0 + 512], a[:, :]), reads=[r_a], writes=[r_pT])
                        if th == 0:
                            a2, r_a2 = next_acc()
                            for j in range(16):
                                P.op("tensor", lambda e, a2=a2, wt=wt, c=c, j=j: e.matmul(
                                    a2[:, 0:HL], wt[:, j, c * 128:(c + 1) * 128], hT[:, j, 0:HL], start=(j == 0), stop=(j == 15)),
                                    reads=[r_wt, r_hTt[0]], writes=[r_a2])
                            P.op("vector", lambda e, a2=a2, c=c: e.tensor_tensor(pT[:, c, 0:HL], a2[:, 0:HL], hmask[:, c, :], ALU.mult),
                                 reads=[r_a2, r_hmask], writes=[r_pT])
                    elif kind == "u":
                        P.op("scalar", lambda e, a=a, c=c, th=th: e.activation(guT[:, c, th * 512:(th + 1) * 512], a[:, :], AF.Gelu_apprx_tanh),
                             reads=[r_a], writes=[r_guT])
                    else:
                        head = (col - (1536 if kind == "q" else 2560)) // 128
                        slot = head + (0 if kind == "q" else 8)
                        gcol = 0 if kind == "q" else 1
                        i = cnt[0] % 2
                        cnt[0] += 1
                        sqt, r_sq = sq[i]
                        rwt, r_rw = raw[i]
                        rs, r_rs = rst[i]
                        P.op("vector", lambda e, a=a, rwt=rwt: e.tensor_copy(rwt[:], a[:, :]), reads=[r_a], writes=[r_rw])
                        P.op("scalar", lambda e, rwt=rwt, sqt=sqt: e.activation(sqt[:], rwt[:], AF.Square), reads=[r_rw], writes=[r_sq])
                        if DBG['qk'] < 1:
                            continue
                        x_, r_x = next_aux()
                        P.op("tensor", lambda e, x_=x_, sqt=sqt: e.matmul(x_[:, :], ones[:], sqt[:], start=True, stop=True),
                             reads=[r_ones, r_sq], writes=[r_x])
                        if DBG['qk'] < 2:
                            continue
                        P.op("scalar", lambda e, x_=x_, rs=rs: e.activation(rs[:], x_[:, :], AF.Sqrt, bias=EPS, scale=1.0 / 128),
                             reads=[r_x], writes=[r_rs])
                        if DBG['qk'] < 3:
                            continue
                        P.op("vector", lambda e, rs=rs: e.reciprocal(rs[:], rs[:]), reads=[r_rs], writes=[r_rs])
                        if DBG['qk'] < 4:
                            continue
                        P.op("vector", lambda e, rs=rs, rwt=rwt, slot=slot, gcol=gcol, th=th: e.scalar_tensor_tensor(
                            qkT[:, slot, th * 512:(th + 1) * 512], rwt[:], qk[:, gcol:gcol + 1], rs[:], ALU.mult, ALU.mult),
                            reads=[r_rw, r_rs, r_qk], writes=[r_qkT])

        def token_major(gi, wt, r_wt):
            kind, c0 = groups[gi]
            for t in range(NTL):
                a, r_a = next_acc()
                t0 = HL + t * 128
                for j in range(16):
                    P.op("tensor", lambda e, a=a, wt=wt, j=j, t0=t0: e.matmul(
                        a[:, :], hT[:, j, t0:t0 + 128], wt[:, j, :], start=(j == 0), stop=(j == 15)),
                        reads=[r_wt, r_hTt[1 + t]], writes=[r_a])
                if kind == "va":
                    vc = (c0 - 3584)
                    P.op("scalar", lambda e, a=a, t=t, vc=vc: e.copy(vsb[:, t, vc:vc + 512], a[:, :]), reads=[r_a], writes=[r_vsb])
                    if c0 == 3584:
                        a2, r_a2 = next_acc()
                        for j in range(16):
                            P.op("tensor", lambda e, a2=a2, j=j, t0=t0: e.matmul(
                                a2[:, 0:8], hT[:, j, t0:t0 + 128], wfz[:, j, :], start=(j == 0), stop=(j == 15)),
                                reads=[r_wfz, r_hTt[1 + t]], writes=[r_a2])
                        P.op("vector", lambda e, a2=a2, t=t: e.tensor_tensor(fz[:, t, :], a2[:, 0:8], bfB[:], ALU.add),
                             reads=[r_a2, r_bfB], writes=[r_fz])
                else:
                    P.op("scalar", lambda e, a=a: e.activation(gv[:], a[:, :], AF.Gelu_apprx_tanh), reads=[r_a], writes=[r_gv])
                    P.op("vector", lambda e: e.tensor_tensor(gv2[:], gv[:], gv[:], ALU.mult), reads=[r_gv], writes=[r_gv2])
                    P.op("vector", lambda e: e.tensor_reduce(vs4[:, 0:4], gv2[:].rearrange("p (h d) -> p h d", h=4), AX.X, ALU.add),
                         reads=[r_gv2], writes=[r_vs4])
                    P.op("scalar", lambda e: e.activation(vs4[:, 4:8], vs4[:, 0:4], AF.Sqrt, bias=EPS, scale=1.0 / 128), reads=[r_vs4], writes=[r_vs4])
                    P.op("vector", lambda e: e.reciprocal(vs4[:, 4:8], vs4[:, 4:8]), reads=[r_vs4], writes=[r_vs4])
                    P.op("vector", lambda e: e.tensor_tensor(
                        gv2[:].rearrange("p (h d) -> p h d", h=4), gv[:].rearrange("p (h d) -> p h d", h=4),
                        vs4[:, 4:8].unsqueeze(2).to_broadcast([128, 4, 128]), ALU.mult), reads=[r_gv, r_vs4], writes=[r_gv2])
                    P.op("vector", lambda e: e.tensor_tensor(vn[:], gv2[:], vgB[:], ALU.mult), reads=[r_gv2, r_vgB], writes=[r_vn])
                    x_, r_x = next_aux()
                    for h in range(4):
                        P.op("tensor", lambda e, x_=x_, h=h: e.matmul(
                            x_[:, h * 128:(h + 1) * 128], vn[:, h * 128:(h + 1) * 128], wsT[:, h, :], start=True, stop=True),
                            reads=[r_vn, r_wsT], writes=[r_x])
                    P.op("vector", lambda e, x_=x_: e.tensor_tensor(spt[:], x_[:, :], bsB[:], ALU.add), reads=[r_x, r_bsB], writes=[r_spt])
                    P.op("vector", lambda e, t=t: e.tensor_tensor(
                        yab[:, 0:4, t * 128:(t + 1) * 128], spt[:].rearrange("p (h t) -> p h t", h=4),
                        guT[:, :, t * 128:(t + 1) * 128], ALU.mult), reads=[r_spt, r_guT], writes=[r_yab])

        for gi in range(len(groups)):
            if gi >= DBG['ngroups']:
                break
            wt, r_wt = wg[gi % 2]
            if groups[gi][0] in ("p", "q", "k", "u"):
                feature_major(gi, wt, r_wt)
            else:
                token_major(gi, wt, r_wt)
            if gi + 2 < len(groups):
                load_group(gi + 2)
            if gi == 0 and DBG['pool']:
                sa, r_sa = xt[0]
                sb_, r_sb = xt[1]
                ft, r_ft = P.sb("ft", [128, HL], F32)
                for g in range(4):
                    w = 2 ** (g + 1)
                    ddt, r_dd = xn[g // 2]
                    ddg = ddt[:, (g % 2) * NT:(g % 2 + 1) * NT]
                    cur_t, cur_r, cur_g = pT, r_pT, g
                    bufs2 = [(sa, r_sa), (sb_, r_sb)]
                    sh = 1
                    for s in range(g + 1):
                        dst, r_dst = bufs2[s % 2]
                        lo = 2 * sh - 1
                        if cur_t is pT:
                            i0 = pT[:, g, lo:TW]
                            i1 = pT[:, g, lo - sh:TW - sh]
                        else:
                            i0 = cur_t[:, lo:TW]
                            i1 = cur_t[:, lo - sh:TW - sh]
                        P.op("vector", lambda e, dst=dst, i0=i0, i1=i1, lo=lo: e.tensor_tensor(dst[:, lo:TW], i0, i1, ALU.add),
                             reads=[cur_r], writes=[r_dst])
                        cur_t, cur_r = dst, r_dst
                        sh *= 2
                    P.op("vector", lambda e, cur_t=cur_t, g=g, w=w, ddg=ddg: e.scalar_tensor_tensor(
                        ddg, cur_t[:, HL:TW], 1.0 / w, pT[:, g, HL:TW], ALU.mult, ALU.subtract),
                        reads=[cur_r, r_pT], writes=[r_dd])
                    P.op("vector", lambda e, cur_t=cur_t, g=g: e.tensor_tensor(ft[:], cur_t[:, HL:2 * HL], invc[:, g, :], ALU.mult),
                         reads=[cur_r, r_invc], writes=[r_ft])
                    P.op("vector", lambda e, g=g, ddg=ddg: e.tensor_tensor(ddg[:, 0:HL], ft[:], pT[:, g, HL:2 * HL], ALU.subtract),
                         reads=[r_ft, r_pT], writes=[r_dd])
                    for th in range(2):
                        x_, r_x = next_aux()
                        P.op("tensor", lambda e, x_=x_, g=g, th=th, ddg=ddg: e.matmul(
                            x_[:, :], bw[:, g, :], ddg[:, th * 512:(th + 1) * 512], start=True, stop=True),
                            reads=[r_bw, r_dd], writes=[r_x])
                        P.op("scalar", lambda e, x_=x_, g=g, th=th: e.activation(
                            yab[:, 4 + g, th * 512:(th + 1) * 512], x_[:, :], AF.Copy, scale=bsc[:, g:g + 1]),
                            reads=[r_x, r_bsc], writes=[r_yab])

        fa, r_fa = P.sb("fa", [128, NTL * 8], F32)
        fm, r_fm = P.sb("fm", [128, NTL * 8], F32)
        fzf = fz[:].rearrange("p t h -> p (t h)")
        if DBG['logsig']:
            P.op("vector", lambda e: e.scalar_tensor_tensor(fa[:], fzf, -1.0, fzf, ALU.mult, ALU.max), reads=[r_fz], writes=[r_fa])
            P.op("scalar", lambda e: e.activation(fa[:], fa[:], AF.Exp, scale=-1.0), reads=[r_fa], writes=[r_fa])
            P.op("scalar", lambda e: e.activation(fa[:], fa[:], AF.Ln, bias=1.0), reads=[r_fa], writes=[r_fa])
            P.op("vector", lambda e: e.tensor_single_scalar(fm[:], fzf, 0.0, ALU.min), reads=[r_fz], writes=[r_fm])
            P.op("vector", lambda e: e.tensor_tensor(fm[:], fm[:], fa[:], ALU.subtract), reads=[r_fa, r_fm], writes=[r_fm])

        P.dma("sync", lambda e: e.dma_start(out=lfo.rearrange("(t p) h -> p t h", p=128), in_=fm[:].rearrange("p (t h) -> p t h", h=8)), r_fm, reads=[r_fm])
        P.dma("sync", lambda e: e.dma_start(out=qTo.rearrange("h d t -> d h t"), in_=qkT[:, 0:8, :]), r_qkT, reads=[r_qkT])
        P.dma("sync", lambda e: e.dma_start(out=kTo.rearrange("h d t -> d h t"), in_=qkT[:, 8:16, :]), r_qkT, reads=[r_qkT])
        P.dma("sync", lambda e: e.dma_start(out=vo.rearrange("(t p) c -> p t c", p=128), in_=vsb[:]), r_vsb, reads=[r_vsb])
        P.dma("sync", lambda e: e.dma_start(out=yabo.rearrange("c f t -> f c t"), in_=yab[:]), r_yab, reads=[r_yab])
        P.finish()
        P.emit()
    return nc


NQG = SEQ // 512
NKB = SEQ // 128


def build_B():
    nc = bass.Bass("TRN2", target_bir_lowering=False)
    I_ = "ExternalInput"
    qTd = _dram(nc, "qT", [128, SEQ], BF16, I_)
    kTd = _dram(nc, "kT", [128, SEQ], BF16, I_)
    vd = _dram(nc, "v", [SEQ, 128], BF16, I_)
    lfd = _dram(nc, "logf", [1, SEQ], F32, I_)
    maskd = _dram(nc, "maskT", [128, 128], F32, I_)
    identd = _dram(nc, "ident", [128, 128], F32, I_)
    yTo = _dram(nc, "yT", [128, SEQ], BF16, "ExternalOutput")
    with ExitStack() as st:
        P = Prog(nc, st)
        qT, r_qT = P.sb("qTs", [128, SEQ], BF16)
        kT, r_kT = P.sb("kTs", [128, SEQ], BF16)
        Vt, r_Vt = P.sb("Vt", [128, NKB, 130], BF16)
        lf, r_lf = P.sb("lf", [1, SEQ], F32)
        Fr, r_Fr = P.sb("Fr", [1, SEQ], F32)
        onesr, r_onesr = P.sb("onesr", [1, SEQ], F32)
        maskT, r_mask = P.sb("maskTs", [128, 128], F32)
        ident, r_ident = P.sb("identb", [128, 128], BF16)
        negF, r_negF = P.sb("negF", [128, NKB], F32)
        yT, r_yT = P.sb("yTs", [128, SEQ], BF16)
        P.op("vector", lambda e: e.memset(Vt[:], 1.0), writes=[r_Vt])
        P.op("vector", lambda e: e.memset(onesr[:], 1.0), writes=[r_onesr])
        P.dma("sync", lambda e: e.dma_start(out=lf[:], in_=lfd), r_lf, writes=[r_lf])
        P.dma("sync", lambda e: e.dma_start(out=maskT[:], in_=maskd), r_mask, writes=[r_mask])
        P.dma("gpsimd", lambda e: e.dma_start(out=ident[:], in_=identd), r_ident, writes=[r_ident])
        for q in range(4):
            sl = slice(q * 2048, (q + 1) * 2048)
            P.dma("sync", lambda e, sl=sl: e.dma_start(out=kT[:, sl], in_=kTd[:, sl]), r_kT, writes=[r_kT])
            P.dma("sync", lambda e, sl=sl: e.dma_start(out=qT[:, sl], in_=qTd[:, sl]), r_qT, writes=[r_qT])
        vr = vd.rearrange("(j p) d -> p j d", p=128)
        for q in range(4):
            P.dma("gpsimd", lambda e, q=q: e.dma_start(out=Vt[:, q * 16:(q + 1) * 16, 0:128], in_=vr[:, q * 16:(q + 1) * 16, :]),
                  r_Vt, writes=[r_Vt])
        P.op("vector", lambda e: e.tensor_tensor_scan(Fr[:], onesr[:], lf[:], 0.0, ALU.mult, ALU.add),
             reads=[r_onesr, r_lf], writes=[r_Fr])
        misc, r_misc = P.ps("misc", [128, 512], F32)
        tpp, r_tpp = P.ps("tpp", [128, 512], BF16)
        for J in range(NKB):
            P.op("tensor", lambda e, J=J: e.matmul(misc[:, J:J + 1], Fr[0:1, J * 128:(J + 1) * 128], onesr[0:1, 0:1], start=True, stop=True),
                 reads=[r_Fr, r_onesr], writes=[r_misc])
        P.op("vector", lambda e: e.tensor_scalar(negF[:], misc[:, 0:NKB], -1.0, None, ALU.mult), reads=[r_misc], writes=[r_negF])

        ps_s = [P.ps(f"ps_s{i}", [128, 512], F32) for i in range(2)]
        ps_o = [P.ps(f"ps_o{i}", [128, 512], F32) for i in range(4)]
        FqB = [P.sb(f"FqB{i}", [128, 512], F32) for i in range(2)]
        tmp = [P.sb(f"tmp{i}", [128, 512], F32) for i in range(3)]
        PT = [P.sb(f"PT{i}", [128, 512], BF16) for i in range(3)]
        rec, r_rec = P.sb("rec", [128, 4], F32)
        yo = [P.sb(f"yo{i}", [128, 128], BF16) for i in range(2)]

        tiles = []
        for I in range(NQG):
            for J in range(4 * I + 4):
                tiles.append((I, J))

        def emit_fq(I):
            fq, r_fq = FqB[I % 2]
            P.op("tensor", lambda e, I=I: e.matmul(misc[:, :], onesr[0:1, 0:128], Fr[0:1, I * 512:(I + 1) * 512], start=True, stop=True),
                 reads=[r_Fr, r_onesr], writes=[r_misc])
            P.op("vector", lambda e, fq=fq: e.tensor_copy(fq[:], misc[:, :]), reads=[r_misc], writes=[r_fq])

        def emit_S(idx):
            I, J = tiles[idx]
            if J == 0:
                emit_fq(I)
            r = J - 4 * I
            c0 = max(r, 0) * 128
            N = 512 - c0
            s_, r_s = ps_s[idx % 2]
            t_, r_t = tmp[idx % 3]
            p_, r_p = PT[idx % 3]
            fq, r_fq = FqB[I % 2]
            P.op("tensor", lambda e, s_=s_, I=I, J=J, c0=c0, N=N: e.matmul(
                s_[:, 0:N], kT[:, J * 128:(J + 1) * 128], qT[:, I * 512 + c0:(I + 1) * 512], start=True, stop=True),
                reads=[r_kT, r_qT], writes=[r_s])
            P.op("vector", lambda e, s_=s_, t_=t_, fq=fq, c0=c0, N=N: e.tensor_tensor(t_[:, 0:N], s_[:, 0:N], fq[:, c0:512], ALU.add),
                 reads=[r_s, r_fq], writes=[r_t])
            if r >= 0:
                P.op("gpsimd", lambda e, t_=t_: e.tensor_tensor(t_[:, 0:128], t_[:, 0:128], maskT[:], ALU.add),
                     reads=[r_t, r_mask], writes=[r_t])
            P.op("scalar", lambda e, t_=t_, p_=p_, J=J, N=N: e.activation(p_[:, 0:N], t_[:, 0:N], AF.Exp, bias=negF[:, J:J + 1]),
                 reads=[r_t, r_negF], writes=[r_p])

        def emit_PV(idx):
            I, J = tiles[idx]
            r = J - 4 * I
            cs = max(r, 0)
            p_, r_p = PT[idx % 3]
            for c in range(cs, 4):
                o_, r_o = ps_o[c]
                P.op("tensor", lambda e, o_=o_, p_=p_, c=c, cs=cs, J=J, I=I: e.matmul(
                    o_[:, 0:129], p_[:, (c - cs) * 128:(c - cs + 1) * 128], Vt[:, J, 0:129], start=(J == 0), stop=(J == 4 * I + c)),
                    reads=[r_p, r_Vt], writes=[r_o])
            if J == 4 * I + 3:
                for c in range(4):
                    o_, r_o = ps_o[c]
                    y_, r_y = yo[c % 2]
                    P.op("vector", lambda e, o_=o_, c=c: e.reciprocal(rec[:, c:c + 1], o_[:, 128:129]), reads=[r_o], writes=[r_rec])
                    P.op("vector", lambda e, o_=o_, y_=y_, c=c: e.tensor_scalar(y_[:], o_[:, 0:128], rec[:, c:c + 1], None, ALU.mult),
                         reads=[r_o, r_rec], writes=[r_y])
                    P.op("tensor", lambda e, y_=y_, c=c: e.transpose(tpp[:, c * 128:(c + 1) * 128], y_[:], ident[:]),
                         reads=[r_y, r_ident], writes=[r_tpp])
                P.op("scalar", lambda e, I=I: e.copy(yT[:, I * 512:(I + 1) * 512], tpp[:, :]), reads=[r_tpp], writes=[r_yT])

        emit_S(0)
        for idx in range(len(tiles)):
            if idx + 1 < len(tiles):
                emit_S(idx + 1)
            emit_PV(idx)
        for q in range(4):
            sl = slice(q * 2048, (q + 1) * 2048)
            P.dma("sync", lambda e, sl=sl: e.dma_start(out=yTo[:, sl], in_=yT[:, sl]), r_yT, reads=[r_yT])
        P.finish()
        P.emit()
    return nc


def build_C():
    nc = bass.Bass("TRN2", target_bir_lowering=False)
    I_ = "ExternalInput"
    x = _dram(nc, "x", [NT, D], F32, I_)
    yTd = _dram(nc, "yT", [16, 128, NT], BF16, I_)
    woutd = _dram(nc, "wout", [D, D], F32, I_)
    modc = _dram(nc, "modc", [128, 3, 16], F32, I_)
    g1d = _dram(nc, "g1", [1, D], F32, I_)
    g2d = _dram(nc, "g2", [1, D], F32, I_)
    wrd = _dram(nc, "wr", [D, NE], F32, I_)
    brd = _dram(nc, "br", [1, NE], F32, I_)
    egd = _dram(nc, "eg", [NE, D, DE], F32, I_)
    eud = _dram(nc, "eu", [NE, D, DE], F32, I_)
    edd = _dram(nc, "ed", [NE, DE, D], F32, I_)
    identd = _dram(nc, "ident", [128, 128], F32, I_)
    xo = _dram(nc, "xo", [NT, D], F32, "ExternalOutput")
    x1s = _dram(nc, "x1s", [NT, D], F32, "Internal")
    with ExitStack() as st:
        P = Prog(nc, st)
        r_x1s = P.res("x1s")
        ident, r_ident = P.sb("identf", [128, 128], F32)
        P.dma("sync", lambda e: e.dma_start(out=ident[:], in_=identd), r_ident, writes=[r_ident])
        mc, r_mc = P.sb("mc", [128, 3, 16], F32)
        P.dma("sync", lambda e: e.dma_start(out=mc[:], in_=modc), r_mc, writes=[r_mc])
        a2, r_a2 = P.sb("a2", [128, 16], F32)
        P.op("vector", lambda e: e.scalar_tensor_tensor(a2[:], mc[:, 1, :], 1.0, mc[:, 2, :], ALU.add, ALU.mult),
             reads=[r_mc], writes=[r_a2])
        GB, r_GB = P.sb("GB", [128, D], F32)
        P.dma("sync", lambda e: e.dma_start(out=GB[:], in_=g1d.partition_broadcast(128)), r_GB, writes=[r_GB])
        wr, r_wr = P.sb("wrs", [128, 16, NE], F32)
        P.dma("sync", lambda e: e.dma_start(out=wr[:], in_=wrd.rearrange("(j p) n -> p j n", p=128)), r_wr, writes=[r_wr])
        brB, r_brB = P.sb("brB", [128, NE], F32)
        P.dma("sync", lambda e: e.dma_start(out=brB[:], in_=brd.partition_broadcast(128)), r_brB, writes=[r_brB])

        acc, r_accb = P.sb("acc", [128, NTL, D], F32)
        r_acc = [P.res(f"acc{t}") for t in range(NTL)]
        arena, _ = P.sb("arena", [128, 32768], BF16)
        yT = arena[:, 0:16384].rearrange("p (j t) -> p j t", j=16)
        r_yT = P.res("yT")
        wo = [arena[:, 16384 + i * 8192:16384 + (i + 1) * 8192].rearrange("p (j n) -> p j n", j=16) for i in range(2)]
        r_wo = [P.res(f"wo{i}") for i in range(2)]
        h2T, r_h2Tb = P.sb("h2T", [128, 16, NT], BF16)
        r_h2T = [P.res(f"h2T{t}") for t in range(NTL)]
        h32, r_h32 = P.sb("h32", [128, 16, 128], F32)
        xn, r_xn = P.sb("xn", [128, D], F32)
        xp = [P.sb(f"xp{i}", [128, 512], F32) for i in range(2)]
        gates, r_gates = P.sb("gates", [128, NTL, NE], F32)
        pb = [P.ps(f"pb{i}", [128, 512], F32) for i in range(8)]

        yTr = yTd.rearrange("c f t -> f c t")
        for q in range(4):
            P.dma("sync", lambda e, q=q: e.dma_start(out=yT[:, q * 4:(q + 1) * 4, :], in_=yTr[:, q * 4:(q + 1) * 4, :]), r_yT, writes=[r_yT])
        woutr = woutd.rearrange("(j p) n -> p j n", p=128)

        def load_wo(cg):
            w_, r_w = wo[cg % 2], r_wo[cg % 2]
            for q in range(4):
                P.dma("gpsimd", lambda e, w_=w_, cg=cg, q=q: e.dma_start(
                    out=w_[:, q * 4:(q + 1) * 4, :], in_=woutr[:, q * 4:(q + 1) * 4, cg * 512:(cg + 1) * 512]), r_w, writes=[r_w])

        load_wo(0)
        load_wo(1)
        k = 0
        for cg in range(4):
            w_, r_w = wo[cg % 2], r_wo[cg % 2]
            for t in range(NTL):
                xpt, r_xp = xp[k % 2]
                ps_, r_ps = pb[k % 2]
                k += 1
                P.dma("sync", lambda e, xpt=xpt, t=t, cg=cg: e.dma_start(out=xpt[:], in_=x[t * 128:(t + 1) * 128, cg * 512:(cg + 1) * 512]),
                      r_xp, writes=[r_xp])
                for j in range(16):
                    P.op("tensor", lambda e, ps_=ps_, w_=w_, j=j, t=t: e.matmul(
                        ps_[:, :], yT[:, j, t * 128:(t + 1) * 128], w_[:, j, :], start=(j == 0), stop=(j == 15)),
                        reads=[r_yT, r_w], writes=[r_ps])
                P.op("vector", lambda e, ps_=ps_, t=t, cg=cg: e.tensor_tensor(
                    acc[:, t, cg * 512:(cg + 1) * 512], ps_[:, :], GB[:, cg * 512:(cg + 1) * 512], ALU.mult),
                    reads=[r_ps, r_GB], writes=[r_acc[t]])
                P.op("gpsimd", lambda e, xpt=xpt, t=t, cg=cg: e.tensor_tensor(
                    acc[:, t, cg * 512:(cg + 1) * 512], acc[:, t, cg * 512:(cg + 1) * 512], xpt[:], ALU.add),
                    reads=[r_xp, r_acc[t]], writes=[r_acc[t]])
            if cg + 2 < 4:
                load_wo(cg + 2)
        tok_p1 = P.engs["tensor"].sem, P.engs["tensor"].count

        ss, r_ss = P.sb("ss", [128, 2], F32)
        rt = {n: P.sb("rt_" + n, [128, NE], F32) for n in ("sc", "sel", "eq", "sel2", "selm", "e1", "e2")}
        r4 = {n: P.sb("r4_" + n, [128, 4], F32) for n in ("m1", "m2", "gs", "gmask", "pen")}
        r1 = {n: P.sb("r1_" + n, [128, 1], F32) for n in ("gm", "t1", "t2", "den")}
        for t in range(NTL):
            P.dma("sync", lambda e, t=t: e.dma_start(out=x1s[t * 128:(t + 1) * 128, :], in_=acc[:, t, :]), r_acc[t],
                  reads=[r_acc[t]], writes=[r_x1s])
            P.op("vector", lambda e: e.memset(ss[:], 0.0), writes=[r_ss])
            P.op("scalar", lambda e, t=t: e.activation(xn[:], acc[:, t, :], AF.Square, accum_out=ss[:, 0:1]),
                 reads=[r_acc[t], r_ss], writes=[r_xn, r_ss])
            P.op("scalar", lambda e: e.activation(ss[:, 1:2], ss[:, 0:1], AF.Sqrt, bias=EPS, scale=1.0 / D), reads=[r_ss], writes=[r_ss])
            P.op("vector", lambda e: e.reciprocal(ss[:, 1:2], ss[:, 1:2]), reads=[r_ss], writes=[r_ss])
            P.op("vector", lambda e, t=t: e.tensor_scalar(xn[:], acc[:, t, :], ss[:, 1:2], None, ALU.mult),
                 reads=[r_acc[t], r_ss], writes=[r_xn])
            for q in range(4):
                ps_, r_ps = pb[2 + (q % 2)]
                for jj in range(4):
                    j = q * 4 + jj
                    P.op("tensor", lambda e, ps_=ps_, j=j, jj=jj: e.transpose(ps_[:, jj * 128:(jj + 1) * 128], xn[:, j * 128:(j + 1) * 128], ident[:]),
                         reads=[r_xn, r_ident], writes=[r_ps])
                for jj in range(4):
                    j = q * 4 + jj
                    if q % 2 == 0:
                        P.op("vector", lambda e, ps_=ps_, j=j, jj=jj: e.tensor_scalar(
                            h32[:, j, :], ps_[:, jj * 128:(jj + 1) * 128], a2[:, j:j + 1], mc[:, 0, j:j + 1], ALU.mult, ALU.add),
                            reads=[r_ps, r_a2, r_mc], writes=[r_h32])
                    else:
                        P.op("scalar", lambda e, ps_=ps_, j=j, jj=jj: e.activation(
                            h32[:, j, :], ps_[:, jj * 128:(jj + 1) * 128], AF.Identity, bias=mc[:, 0, j:j + 1], scale=a2[:, j:j + 1]),
                            reads=[r_ps, r_a2, r_mc], writes=[r_h32])
            P.op("gpsimd", lambda e, t=t: e.tensor_copy(h2T[:, :, t * 128:(t + 1) * 128], h32[:]), reads=[r_h32], writes=[r_h2T[t]])
            lg, r_lg = pb[4]
            for j in range(16):
                P.op("tensor", lambda e, j=j: e.matmul(lg[:, 0:NE], h32[:, j, :], wr[:, j, :], start=(j == 0), stop=(j == 15)),
                     reads=[r_h32, r_wr], writes=[r_lg])
            sc, r_sc = rt["sc"]; sel, r_sel = rt["sel"]; eq, r_eq = rt["eq"]; sel2, r_sel2 = rt["sel2"]
            selm, r_selm = rt["selm"]; e1, r_e1 = rt["e1"]; e2, r_e2 = rt["e2"]
            m1, r_m1 = r4["m1"]; m2, r_m2 = r4["m2"]; gs, r_gs = r4["gs"]; gmask, r_gmask = r4["gmask"]; pen, r_pen = r4["pen"]
            gm, r_gm = r1["gm"]; t1, r_t1 = r1["t1"]; t2, r_t2 = r1["t2"]; den, r_den = r1["den"]
            g3 = lambda ap: ap.rearrange("p (g e) -> p g e", g=4)
            b3 = lambda ap: ap.unsqueeze(2).to_broadcast([128, 4, 4])
            P.op("scalar", lambda e: e.activation(sc[:], lg[:, 0:NE], AF.Sigmoid), reads=[r_lg], writes=[r_sc])
            P.op("vector", lambda e: e.tensor_tensor(sel[:], sc[:], brB[:], ALU.add), reads=[r_sc, r_brB], writes=[r_sel])
            P.op("vector", lambda e: e.tensor_reduce(m1[:], g3(sel[:]), AX.X, ALU.max), reads=[r_sel], writes=[r_m1])
            P.op("vector", lambda e: e.tensor_tensor(g3(eq[:]), g3(sel[:]), b3(m1[:]), ALU.is_equal), reads=[r_sel, r_m1], writes=[r_eq])
            P.op("vector", lambda e: e.scalar_tensor_tensor(sel2[:], eq[:], -1e9, sel[:], ALU.mult, ALU.add), reads=[r_eq, r_sel], writes=[r_sel2])
            P.op("vector", lambda e: e.tensor_reduce(m2[:], g3(sel2[:]), AX.X, ALU.max), reads=[r_sel2], writes=[r_m2])
            P.op("vector", lambda e: e.tensor_tensor(gs[:], m1[:], m2[:], ALU.add), reads=[r_m1, r_m2], writes=[r_gs])
            P.op("vector", lambda e: e.tensor_reduce(gm[:], gs[:], AX.X, ALU.max), reads=[r_gs], writes=[r_gm])
            P.op("vector", lambda e: e.tensor_scalar(gmask[:], gs[:], gm[:, 0:1], None, ALU.is_equal), reads=[r_gs, r_gm], writes=[r_gmask])
            P.op("vector", lambda e: e.tensor_scalar(pen[:], gmask[:], 1e9, -1e9, ALU.mult, ALU.add), reads=[r_gmask], writes=[r_pen])
            P.op("vector", lambda e: e.tensor_tensor(g3(selm[:]), g3(sel[:]), b3(pen[:]), ALU.add), reads=[r_sel, r_pen], writes=[r_selm])
            P.op("vector", lambda e: e.tensor_reduce(t1[:], selm[:], AX.X, ALU.max), reads=[r_selm], writes=[r_t1])
            P.op("vector", lambda e: e.tensor_scalar(e1[:], selm[:], t1[:, 0:1], None, ALU.is_equal), reads=[r_selm, r_t1], writes=[r_e1])
            P.op("vector", lambda e: e.scalar_tensor_tensor(sel2[:], e1[:], -1e9, selm[:], ALU.mult, ALU.add), reads=[r_e1, r_selm], writes=[r_sel2])
            P.op("vector", lambda e: e.tensor_reduce(t2[:], sel2[:], AX.X, ALU.max), reads=[r_sel2], writes=[r_t2])
            P.op("vector", lambda e: e.tensor_scalar(e2[:], sel2[:], t2[:, 0:1], None, ALU.is_equal), reads=[r_sel2, r_t2], writes=[r_e2])
            P.op("vector", lambda e: e.tensor_tensor(e1[:], e1[:], e2[:], ALU.add), reads=[r_e1, r_e2], writes=[r_e1])
            P.op("vector", lambda e: e.tensor_tensor(e1[:], e1[:], sc[:], ALU.mult), reads=[r_e1, r_sc], writes=[r_e1])
            P.op("vector", lambda e: e.tensor_reduce(den[:], e1[:], AX.X, ALU.add), reads=[r_e1], writes=[r_den])
            P.op("vector", lambda e: e.reciprocal(den[:], den[:]), reads=[r_den], writes=[r_den])
            P.op("vector", lambda e, t=t: e.tensor_scalar(gates[:, t, :], e1[:], den[:, 0:1], None, ALU.mult), reads=[r_e1, r_den], writes=[r_gates])

        P.dma("sync", lambda e: e.dma_start(out=GB[:], in_=g2d.partition_broadcast(128)), r_GB, writes=[r_GB])

        hid = arena[:, 0:8192].rearrange("p (j t) -> p j t", j=8)
        r_hid = [P.res(f"hid{i}") for i in range(2)]
        gsl = [arena[:, 8192 + i * 4096:8192 + (i + 1) * 4096].rearrange("p (j n) -> p j n", j=16) for i in range(4)]
        r_gsl = [P.res(f"gsl{i}") for i in range(4)]
        dsl = [arena[:, 24576 + i * 4096:24576 + (i + 1) * 4096].rearrange("p (j n) -> p j n", j=8) for i in range(2)]
        r_dsl = [P.res(f"dsl{i}") for i in range(2)]
        for r in r_hid + r_gsl + r_dsl:
            r.readers.append(tok_p1)
        sg = [P.sb(f"sg{i}", [128, 512], F32) for i in range(2)]
        egr = egd.rearrange("e (j p) f -> e p j f", p=128)
        eur = eud.rearrange("e (j p) f -> e p j f", p=128)
        edr = edd.rearrange("e (j p) n -> e p j n", p=128)

        gu_jobs = [(e_, fp) for e_ in range(NE) for fp in range(4)]
        d_jobs = [(e_, cb) for e_ in range(NE) for cb in range(4)]

        def load_gu(i):
            e_, fp = gu_jobs[i]
            for which, src in ((0, egr), (1, eur)):
                b = (i % 2) * 2 + which
                for q in range(2):
                    P.dma("gpsimd", lambda e, b=b, src=src, e_=e_, fp=fp, q=q: e.dma_start(
                        out=gsl[b][:, q * 8:(q + 1) * 8, :], in_=src[e_, :, q * 8:(q + 1) * 8, fp * 256:(fp + 1) * 256]),
                        r_gsl[b], writes=[r_gsl[b]])

        def load_d(i):
            e_, cb = d_jobs[i]
            b = i % 2
            P.dma("gpsimd", lambda e, b=b, e_=e_, cb=cb: e.dma_start(out=dsl[b][:], in_=edr[e_, :, :, cb * 512:(cb + 1) * 512]),
                  r_dsl[b], writes=[r_dsl[b]])

        load_gu(0)
        load_gu(1)
        load_d(0)
        load_d(1)
        kk = 0
        dk = 0
        for e_ in range(NE):
            for fp in range(4):
                i = e_ * 4 + fp
                for fs in range(2):
                    fc = fp * 2 + fs
                    for th in range(2):
                        pg, r_pg = pb[(kk % 2) * 2]
                        pu, r_pu = pb[(kk % 2) * 2 + 1]
                        sgt, r_sg = sg[kk % 2]
                        kk += 1
                        bg = (i % 2) * 2
                        for j in range(16):
                            P.op("tensor", lambda e, pg=pg, bg=bg, fs=fs, j=j, th=th: e.matmul(
                                pg[:, :], gsl[bg][:, j, fs * 128:(fs + 1) * 128], h2T[:, j, th * 512:(th + 1) * 512],
                                start=(j == 0), stop=(j == 15)), reads=[r_gsl[bg]] + r_h2T[th * 4:th * 4 + 4], writes=[r_pg])
                        for j in range(16):
                            P.op("tensor", lambda e, pu=pu, bg=bg, fs=fs, j=j, th=th: e.matmul(
                                pu[:, :], gsl[bg + 1][:, j, fs * 128:(fs + 1) * 128], h2T[:, j, th * 512:(th + 1) * 512],
                                start=(j == 0), stop=(j == 15)), reads=[r_gsl[bg + 1]] + r_h2T[th * 4:th * 4 + 4], writes=[r_pu])
                        P.op("scalar", lambda e, pg=pg, sgt=sgt: e.activation(sgt[:], pg[:, :], AF.Silu), reads=[r_pg], writes=[r_sg])
                        P.op("vector", lambda e, pu=pu, sgt=sgt, fc=fc, th=th: e.tensor_tensor(
                            hid[:, fc, th * 512:(th + 1) * 512], sgt[:], pu[:, :], ALU.mult), reads=[r_sg, r_pu], writes=[r_hid[th]])
                if i + 2 < len(gu_jobs):
                    load_gu(i + 2)
            for cb in range(4):
                di = e_ * 4 + cb
                b = di % 2
                for t in range(NTL):
                    po, r_po = pb[4 + dk % 4]
                    dk += 1
                    for j8 in range(8):
                        P.op("tensor", lambda e, po=po, b=b, j8=j8, t=t: e.matmul(
                            po[:, :], hid[:, j8, t * 128:(t + 1) * 128], dsl[b][:, j8, :], start=(j8 == 0), stop=(j8 == 7)),
                            reads=[r_hid[t // 4], r_dsl[b]], writes=[r_po])
                    sl = slice(cb * 512, (cb + 1) * 512)
                    if e_ == 0:
                        P.op("vector", lambda e, po=po, t=t, sl=sl, e_=e_: e.tensor_scalar(
                            acc[:, t, sl], po[:, :], gates[:, t, e_:e_ + 1], None, ALU.mult),
                            reads=[r_po, r_gates], writes=[r_acc[t]])
                    else:
                        P.op("vector", lambda e, po=po, t=t, sl=sl, e_=e_: e.scalar_tensor_tensor(
                            acc[:, t, sl], po[:, :], gates[:, t, e_:e_ + 1], acc[:, t, sl], ALU.mult, ALU.add),
                            reads=[r_po, r_gates, r_acc[t]], writes=[r_acc[t]])
                if di + 2 < len(d_jobs):
                    load_d(di + 2)

        for t in range(NTL):
            P.dma("sync", lambda e, t=t: e.dma_start(out=xn[:], in_=x1s[t * 128:(t + 1) * 128, :]), r_xn, reads=[r_x1s], writes=[r_xn])
            P.op("vector", lambda e, t=t: e.tensor_tensor(acc[:, t, :], acc[:, t, :], GB[:], ALU.mult), reads=[r_acc[t], r_GB], writes=[r_acc[t]])
            P.op("gpsimd", lambda e, t=t: e.tensor_tensor(acc[:, t, :], acc[:, t, :], xn[:], ALU.add), reads=[r_acc[t], r_xn], writes=[r_acc[t]])
            P.dma("sync", lambda e, t=t: e.dma_start(out=xo[t * 128:(t + 1) * 128, :], in_=acc[:, t, :]), r_acc[t], reads=[r_acc[t]])
        P.finish()
        P.emit()
    return nc


_BF = ml_dtypes.bfloat16
_CORES = list(range(NCORES))
_PROGS = {}


def _prog(name, builder):
    if name not in _PROGS:
        _PROGS[name] = builder()
    return _PROGS[name]


def _col(v):
    return np.ascontiguousarray(np.asarray(v, np.float32).reshape(16, 128).T)


def _run(nc, maps):
    return run_bass_kernel_spmd(nc, maps, core_ids=_CORES).results


def kernel(x, c, w_ada, b_ada, g_mix, g_ffn, w_in, a_ws, a_bs, a_vg, b_w, b_scale,
           c_qg, c_kg, c_bf, w_out, w_router, b_router, e_gate, e_up, e_down):
    f32 = lambda a: np.asarray(a, dtype=np.float32)
    x = f32(x)[0]
    c = f32(c)
    w_ada, b_ada = f32(w_ada), f32(b_ada)
    ident = np.eye(128, dtype=np.float32)
    maskT = np.where(np.arange(128)[None, :] >= np.arange(128)[:, None], 0.0, NEG).astype(np.float32)

    ccol = _col(c[0])
    res = _run(_prog("mod", build_mod), [
        {"ccol": ccol, "wada": np.ascontiguousarray(w_ada[:, :, i * MODC:(i + 1) * MODC]),
         "bada": np.ascontiguousarray(b_ada[:, i * MODC:(i + 1) * MODC])} for i in _CORES])
    mod = np.concatenate([np.asarray(r["modo"]) for r in res], axis=1)

    for l in range(2):
        m = mod[l]
        seg = lambda i: m[i * D:(i + 1) * D]
        modcA = np.ascontiguousarray(np.stack([_col(seg(0)), _col(seg(1)), _col(f32(g_mix)[l])], axis=1))
        w_in_l = f32(w_in)[l]
        awsT = np.ascontiguousarray(f32(a_ws)[l].transpose(0, 2, 1))
        abs_ = f32(a_bs)[l].reshape(1, 512)
        avg_ = f32(a_vg)[l].reshape(1, 512)
        bw_l = f32(b_w)[l]
        bsc = np.ascontiguousarray(f32(b_scale)[l].reshape(4, 128).T)
        qkg = np.ascontiguousarray(np.stack([f32(c_qg)[l], f32(c_kg)[l]], axis=1))
        cbf = f32(c_bf)[l].reshape(1, 8)
        maps = []
        for i in _CORES:
            s = i * NT
            xh = x[s - HL:s] if i > 0 else np.zeros((HL, D), np.float32)
            hmask = np.full((128, 4, HL), 0.0 if i == 0 else 1.0, np.float32)
            invc = np.zeros((128, 4, HL), np.float32)
            for gi, w in enumerate((2, 4, 8, 16)):
                for t in range(HL):
                    invc[:, gi, t] = 1.0 / min(s + t + 1, w)
            maps.append({"x": np.ascontiguousarray(x[s:s + NT]), "xh": np.ascontiguousarray(xh), "modc": modcA,
                         "win": w_in_l, "ident": ident, "awsT": awsT, "abs": abs_, "avg": avg_, "bw": bw_l,
                         "bsc": bsc, "qkg": qkg, "cbf": cbf, "hmask": hmask, "invc": invc})
        ra = _run(_prog("A", build_A), maps)
        qT = np.concatenate([np.asarray(r["qT"]) for r in ra], axis=2)
        kT = np.concatenate([np.asarray(r["kT"]) for r in ra], axis=2)
        v = np.concatenate([np.asarray(r["v"]) for r in ra], axis=0)
        logf = np.concatenate([np.asarray(r["logf"]) for r in ra], axis=0)
        maps = [{"qT": np.ascontiguousarray(qT[h]), "kT": np.ascontiguousarray(kT[h]),
                 "v": np.ascontiguousarray(v[:, h * 128:(h + 1) * 128]),
                 "logf": np.ascontiguousarray(logf[:, h].reshape(1, SEQ)), "maskT": maskT, "ident": ident} for h in _CORES]
        rb = _run(_prog("B", build_B), maps)
        modcC = np.ascontiguousarray(np.stack([_col(seg(3)), _col(seg(4)), _col(f32(g_ffn)[l])], axis=1))
        w_out_l = f32(w_out)[l]
        wr = f32(w_router)
        br = f32(b_router).reshape(1, NE)
        eg, eu, ed = f32(e_gate)[l], f32(e_up)[l], f32(e_down)[l]
        maps = []
        for i in _CORES:
            s = i * NT
            yc = np.stack([np.asarray(rb[h]["yT"])[:, s:s + NT] for h in range(8)], axis=0)
            yT = np.ascontiguousarray(np.concatenate([np.asarray(ra[i]["yab"]), yc], axis=0))
            maps.append({"x": np.ascontiguousarray(x[s:s + NT]), "yT": yT, "wout": w_out_l, "modc": modcC,
                         "g1": np.ascontiguousarray(seg(2).reshape(1, D)), "g2": np.ascontiguousarray(seg(5).reshape(1, D)),
                         "wr": wr, "br": br, "eg": eg, "eu": eu, "ed": ed, "ident": ident})
        rc = _run(_prog("C", build_C), maps)
        x = np.concatenate([np.asarray(r["xo"]) for r in rc], axis=0)
    return x[None].astype(np.float32)
```
